# Optimizing a Trainium2 kernel written in Bass

```python
import numpy as np
import jax, jax.numpy as jnp
from jax import lax

D_MODEL = 2048
BATCH = 4
SEQ = 2048
DEPTH = 4

N_EVEN = (DEPTH + 1) // 2
N_ODD = DEPTH // 2

A_HEADS = 8
A_DK = 128
A_DV = 128
A_QK = A_HEADS * A_DK
A_WIDTH = A_HEADS * A_DV
B_HEADS = 8
B_DK = 128
B_DV = 128
B_QK = B_HEADS * B_DK
B_WIDTH = B_HEADS * B_DV
CONV_K = 4
CHUNK = 64
EVEN_IN = 2 * A_QK + 2 * A_WIDTH + 2 * B_QK + 2 * B_WIDTH + 2 * B_HEADS
EVEN_OUT = A_WIDTH + B_WIDTH

C_HEADS = 16
C_GROUPS = 4
C_HPG = C_HEADS // C_GROUPS
C_DK = 128
C_DV = 128
C_Q = C_HEADS * C_DK
C_KV = C_GROUPS * C_DK
CMP_LEN = 32
CMP_STRIDE = 16
SEL_BLOCK = 64
N_SEL = 8
WINDOW = 512
Q_BLOCK = 128
SEL_Q_BLOCK = 64
SEL_FORCE = 1e4
ODD_IN = C_Q + 6 * C_KV + 3 * C_HEADS
ODD_OUT = C_HEADS * C_DV

D_FF = -(-8 * D_MODEL // (3 * 256)) * 256
NORM_EPS = 1e-6
NEG_INF = -1e30

kernel_name = 'hybrid_hgrn2_gdn_nsa_sandwich'


def rmsnorm(x, g):
    xf = x.astype(jnp.float32)
    y = xf * lax.rsqrt(jnp.mean(xf * xf, axis=-1, keepdims=True) + NORM_EPS)
    return (y * g.astype(jnp.float32)).astype(x.dtype)


def l2norm(x):
    return x * lax.rsqrt(jnp.sum(x * x, axis=-1, keepdims=True) + NORM_EPS)


def masked_softmax(s, mask):
    return jax.nn.softmax(jnp.where(mask, s, NEG_INF), axis=-1) * mask


def alibi_slopes(n):
    return jnp.asarray(2.0 ** (-8.0 * np.arange(1, n + 1) / n), jnp.float32)


def _chunks(x):
    b, t, h = x.shape[:3]
    x = x.reshape((b, t // CHUNK, CHUNK, h) + x.shape[3:])
    return jnp.moveaxis(x, 3, 1)


def _unchunk(o):
    n, b, h, c, d = o.shape
    return o.transpose(1, 0, 3, 2, 4).reshape(b, n * c, h, d)


def _causal_conv(x, w):
    return lax.conv_general_dilated(x, w[:, None, :], window_strides=(1,), padding=[(CONV_K - 1, 0)],
                                    dimension_numbers=('NWC', 'WIO', 'NWC'), feature_group_count=x.shape[-1])


def _hgrn2(q, f_logit, i, lb):
    b_, t_, h_, dk = q.shape
    dv = i.shape[-1]
    f = lb + (1.0 - lb) * jax.nn.sigmoid(f_logit)
    log_f = jnp.log(f)
    k = 1.0 - f
    xs = tuple(jnp.moveaxis(_chunks(a), 2, 0) for a in (q, k, i, log_f))
    tril = jnp.tril(jnp.ones((CHUNK, CHUNK), bool))[:, :, None]

    def step(S, inp):
        qn, kn, vn, ln = inp
        b = jnp.cumsum(ln, axis=2)
        decay = jnp.exp(jnp.where(tril, b[:, :, :, None, :] - b[:, :, None, :, :], -jnp.inf))
        intra = jnp.einsum('bhtd,bhsd,bhtsd->bhts', qn, kn, decay)
        o = jnp.einsum('bhts,bhse->bhte', intra, vn) + jnp.einsum('bhtd,bhde->bhte', qn * jnp.exp(b), S)
        b_end = b[:, :, -1:, :]
        S = jnp.exp(b_end[:, :, 0, :, None]) * S + jnp.einsum('bhsd,bhse->bhde', kn * jnp.exp(b_end - b), vn)
        return S, o

    S0 = jnp.zeros((b_, h_, dk, dv), jnp.float32)
    _, o = lax.scan(step, S0, xs)
    return _unchunk(o)


def _gated_delta(q, k, v, beta, log_decay):
    b_, t_, h_, dk = q.shape
    dv = v.shape[-1]
    qc, kc, vc = _chunks(q), _chunks(k), _chunks(v)
    bc = _chunks(beta)
    gc = jnp.cumsum(_chunks(log_decay), axis=-1)
    eye = jnp.eye(CHUNK, dtype=bool)
    tril = jnp.tril(jnp.ones((CHUNK, CHUNK), bool))
    decay = jnp.exp(jnp.where(tril, gc[..., :, None] - gc[..., None, :], -jnp.inf))
    kb = kc * bc[..., None]
    L = jnp.where(tril & ~eye, jnp.einsum('bhncd,bhnsd->bhncs', kb, kc) * decay, 0.0)
    rhs = jnp.concatenate([vc * bc[..., None], kb * jnp.exp(gc)[..., None]], axis=-1)
    sol = lax.linalg.triangular_solve(L + eye, rhs, left_side=True, lower=True, unit_diagonal=True)
    u, w = sol[..., :dv], sol[..., dv:]
    qk = jnp.einsum('bhncd,bhnsd->bhncs', qc, kc) * decay
    xs = tuple(jnp.moveaxis(a, 2, 0) for a in (qc, kc, u, w, qk, gc))

    def step(S, inp):
        qn, kn, un, wn, qkn, gn = inp
        v_new = un - jnp.einsum('bhcd,bhde->bhce', wn, S)
        o = (jnp.einsum('bhcd,bhde->bhce', qn * jnp.exp(gn)[..., None], S)
             + jnp.einsum('bhcs,bhse->bhce', qkn, v_new))
        g_end = gn[..., -1:]
        S = (jnp.exp(g_end)[..., None] * S
             + jnp.einsum('bhcd,bhce->bhde', kn * jnp.exp(g_end - gn)[..., None], v_new))
        return S, o

    S0 = jnp.zeros((b_, h_, dk, dv), jnp.float32)
    _, o = lax.scan(step, S0, xs)
    return _unchunk(o)


def _even_mixer(u, w_in, lb, conv_w, a_log, dt_bias, hgrn_g, gdn_g, w_out):
    b_, t_, _ = u.shape
    z = (u @ w_in).astype(jnp.float32)
    cuts = [int(c) for c in np.cumsum([A_QK, A_QK, A_WIDTH, A_WIDTH, B_QK, B_QK, B_WIDTH, B_WIDTH, B_HEADS])]
    aq, af, ai, ag, bq, bk, bv, bg, ba, bb = jnp.split(z, cuts, axis=-1)
    o_a = _hgrn2(aq.reshape(b_, t_, A_HEADS, A_DK), af.reshape(b_, t_, A_HEADS, A_DK),
                 ai.reshape(b_, t_, A_HEADS, A_DV), lb)
    o_a = rmsnorm(o_a, hgrn_g) * jax.nn.silu(ag.reshape(b_, t_, A_HEADS, A_DV))
    qkv = jax.nn.silu(_causal_conv(jnp.concatenate([bq, bk, bv], axis=-1), conv_w.astype(jnp.float32)))
    bq, bk, bv = jnp.split(qkv, [B_QK, 2 * B_QK], axis=-1)
    q = l2norm(bq.reshape(b_, t_, B_HEADS, B_DK)) * (B_DK ** -0.5)
    k = l2norm(bk.reshape(b_, t_, B_HEADS, B_DK))
    beta = jax.nn.sigmoid(bb)
    log_decay = -jnp.exp(a_log.astype(jnp.float32)) * jax.nn.softplus(ba + dt_bias.astype(jnp.float32))
    o_b = _gated_delta(q, k, bv.reshape(b_, t_, B_HEADS, B_DV), beta, log_decay)
    o_b = rmsnorm(o_b, gdn_g) * jax.nn.silu(bg.reshape(b_, t_, B_HEADS, B_DV))
    o = jnp.concatenate([o_a.reshape(b_, t_, A_WIDTH), o_b.reshape(b_, t_, B_WIDTH)], axis=-1)
    return o.astype(u.dtype) @ w_out


def _nsa_mixer(u, w_in, cmp_pos, cmp_w1, cmp_w2, w_out):
    b_, t_, _ = u.shape
    G, HP = C_GROUPS, C_HPG
    z = (u @ w_in).astype(jnp.float32)
    cuts = [C_Q + j * C_KV for j in range(7)]
    q, kc, vc, ks, vs, kw, vw, gl = jnp.split(z, cuts, axis=-1)
    kv = lambda a: a.reshape(b_, t_, G, C_DK)
    kc, vc, ks, vs, kw, vw = (kv(a) for a in (kc, vc, ks, vs, kw, vw))
    qg = q.reshape(b_, t_, G, HP, C_DK).transpose(0, 2, 3, 1, 4) * (C_DK ** -0.5)
    gates = jax.nn.sigmoid(gl.reshape(b_, t_, G, HP, 3)).transpose(0, 2, 3, 1, 4)
    slopes = alibi_slopes(C_HEADS).reshape(G, HP)
    tpos = jnp.arange(t_)

    n_cmp = (t_ - CMP_LEN) // CMP_STRIDE + 1
    cstart = jnp.arange(n_cmp) * CMP_STRIDE
    cidx = cstart[:, None] + jnp.arange(CMP_LEN)[None, :]

    def compress(a, pos, w1, w2):
        blk = a[:, cidx] + pos[None, None, :, None, :]
        blk = blk.transpose(0, 1, 3, 2, 4).reshape(b_, n_cmp, G, CMP_LEN * C_DK)
        return jax.nn.silu(blk @ w1) @ w2

    k_cmp = compress(kc, cmp_pos[0], cmp_w1[0], cmp_w2[0])
    v_cmp = compress(vc, cmp_pos[1], cmp_w1[1], cmp_w2[1])
    dist_c = tpos[:, None] - (cstart + CMP_LEN - 1)[None, :]
    s_c = jnp.einsum('bghtd,bjgd->bghtj', qg, k_cmp) - slopes[:, :, None, None] * dist_c
    p_c = masked_softmax(s_c, dist_c >= 0)
    o_cmp = jnp.einsum('bghtj,bjge->bghte', p_c, v_cmp)

    n_blk = t_ // SEL_BLOCK
    n_sel = min(N_SEL, n_blk)
    bstart = jnp.arange(n_blk) * SEL_BLOCK
    overlap = ((cstart[:, None] < bstart[None, :] + SEL_BLOCK)
               & (cstart[:, None] + CMP_LEN > bstart[None, :])).astype(jnp.float32)
    imp = jnp.einsum('bghtj,ji->bgti', p_c, overlap)
    cur = (tpos // SEL_BLOCK)[:, None]
    bi = jnp.arange(n_blk)[None, :]
    forced = (bi == 0) | (bi == cur) | (bi == cur - 1)
    score = jnp.where(forced, SEL_FORCE, jnp.where(bi > cur, -SEL_FORCE, imp))
    _, sel_idx = lax.top_k(score, n_sel)

    ks_blocks = ks.reshape(b_, n_blk, SEL_BLOCK, G, C_DK).transpose(0, 3, 1, 2, 4)
    vs_blocks = vs.reshape(b_, n_blk, SEL_BLOCK, G, C_DV).transpose(0, 3, 1, 2, 4)
    nqb = t_ // SEL_Q_BLOCK
    q_b = qg.reshape(b_, G, HP, nqb, SEL_Q_BLOCK, C_DK).transpose(3, 0, 1, 2, 4, 5)
    idx_b = sel_idx.reshape(b_, G, nqb, SEL_Q_BLOCK, n_sel).transpose(2, 0, 1, 3, 4)
    t_b = tpos.reshape(nqb, SEL_Q_BLOCK)
    gather = jax.vmap(jax.vmap(lambda blocks, ix: blocks[ix]))
    n_keys = n_sel * SEL_BLOCK

    def sel_block(args):
        qb, ib, tb = args
        kg = gather(ks_blocks, ib).reshape(b_, G, SEL_Q_BLOCK, n_keys, C_DK)
        vg = gather(vs_blocks, ib).reshape(b_, G, SEL_Q_BLOCK, n_keys, C_DV)
        kpos = (ib[..., None] * SEL_BLOCK + jnp.arange(SEL_BLOCK)).reshape(b_, G, SEL_Q_BLOCK, n_keys)
        dist = tb[None, None, :, None] - kpos
        s = (jnp.einsum('bghqd,bgqkd->bghqk', qb, kg)
             - slopes[None, :, :, None, None] * dist[:, :, None])
        p = masked_softmax(s, (dist >= 0)[:, :, None])
        return jnp.einsum('bghqk,bgqke->bghqe', p, vg)

    o_slc = lax.map(sel_block, (q_b, idx_b, t_b))
    o_slc = o_slc.transpose(1, 2, 3, 0, 4, 5).reshape(b_, G, HP, t_, C_DV)

    nwb = t_ // Q_BLOCK
    band = jnp.arange(nwb)[:, None] * Q_BLOCK + jnp.arange(Q_BLOCK + WINDOW)[None, :]
    pad = ((0, 0), (0, 0), (WINDOW, 0), (0, 0))
    kb = jnp.pad(kw.transpose(0, 2, 1, 3), pad)[:, :, band]
    vb = jnp.pad(vw.transpose(0, 2, 1, 3), pad)[:, :, band]
    kpos = band - WINDOW
    dist_w = tpos.reshape(nwb, Q_BLOCK)[:, :, None] - kpos[:, None, :]
    valid_w = (dist_w >= 0) & (dist_w < WINDOW) & (kpos[:, None, :] >= 0)
    qw = qg.reshape(b_, G, HP, nwb, Q_BLOCK, C_DK)
    s_w = jnp.einsum('bghnqd,bgnkd->bghnqk', qw, kb) - slopes[:, :, None, None, None] * dist_w
    p_w = masked_softmax(s_w, valid_w)
    o_win = jnp.einsum('bghnqk,bgnke->bghnqe', p_w, vb).reshape(b_, G, HP, t_, C_DV)

    o = gates[..., 0:1] * o_cmp + gates[..., 1:2] * o_slc + gates[..., 2:3] * o_win
    o = o.transpose(0, 3, 1, 2, 4).reshape(b_, t_, ODD_OUT)
    return o.astype(u.dtype) @ w_out


def _swiglu(u, w_gu, w_down):
    gate, up = jnp.split(u @ w_gu, 2, axis=-1)
    return (jax.nn.silu(gate) * up) @ w_down


def setup_inputs(seed: int = 0) -> dict:
    key = jax.random.key(seed)
    ks = jax.random.split(key, 20)
    f32 = jnp.float32
    nrm = lambda k, shape: jax.random.normal(k, shape, f32)
    w = lambda k, shape, fan_in: nrm(k, shape) * fan_in ** -0.5
    dt = jnp.exp(jax.random.uniform(ks[6], (N_EVEN, B_HEADS), f32, np.log(1e-3), np.log(1e-1)))
    return {
        'x': nrm(ks[0], (BATCH, SEQ, D_MODEL)),
        'norm_g': 1.0 + 0.05 * nrm(ks[1], (DEPTH, 4, D_MODEL)),
        'ab_w_in': w(ks[2], (N_EVEN, D_MODEL, EVEN_IN), D_MODEL),
        'hgrn_lb_logits': 0.5 * nrm(ks[3], (N_EVEN, A_QK)),
        'gdn_conv_w': w(ks[4], (N_EVEN, CONV_K, 2 * B_QK + B_WIDTH), CONV_K),
        'gdn_a_log': jnp.log(jax.random.uniform(ks[5], (N_EVEN, B_HEADS), f32, 1.0, 16.0)),
        'gdn_dt_bias': dt + jnp.log(-jnp.expm1(-dt)),
        'hgrn_norm_g': 1.0 + 0.05 * nrm(ks[7], (N_EVEN, A_DV)),
        'gdn_norm_g': 1.0 + 0.05 * nrm(ks[8], (N_EVEN, B_DV)),
        'ab_w_out': w(ks[9], (N_EVEN, EVEN_OUT, D_MODEL), EVEN_OUT),
        'nsa_w_in': w(ks[10], (N_ODD, D_MODEL, ODD_IN), D_MODEL),
        'nsa_cmp_pos': 0.1 * nrm(ks[11], (N_ODD, 2, CMP_LEN, C_DK)),
        'nsa_cmp_w1': w(ks[12], (N_ODD, 2, CMP_LEN * C_DK, C_DK), CMP_LEN * C_DK),
        'nsa_cmp_w2': w(ks[13], (N_ODD, 2, C_DK, C_DK), C_DK),
        'nsa_w_out': w(ks[14], (N_ODD, ODD_OUT, D_MODEL), ODD_OUT),
        'ffn_w_gu': w(ks[15], (DEPTH, D_MODEL, 2 * D_FF), D_MODEL),
        'ffn_w_down': w(ks[16], (DEPTH, D_FF, D_MODEL), D_FF),
    }


def reference(x, norm_g, ab_w_in, hgrn_lb_logits, gdn_conv_w, gdn_a_log, gdn_dt_bias, hgrn_norm_g,
              gdn_norm_g, ab_w_out, nsa_w_in, nsa_cmp_pos, nsa_cmp_w1, nsa_cmp_w2, nsa_w_out,
              ffn_w_gu, ffn_w_down):
    lb_all = jnp.cumsum(jax.nn.softmax(hgrn_lb_logits.astype(jnp.float32), axis=0), axis=0)
    lb_all = lb_all - lb_all[:1]
    h = x
    for layer in range(DEPTH):
        j = layer // 2
        u = rmsnorm(h, norm_g[layer, 0])
        if layer % 2 == 0:
            m = _even_mixer(u, ab_w_in[j], lb_all[j].reshape(A_HEADS, A_DK), gdn_conv_w[j], gdn_a_log[j],
                            gdn_dt_bias[j], hgrn_norm_g[j], gdn_norm_g[j], ab_w_out[j])
        else:
            m = _nsa_mixer(u, nsa_w_in[j], nsa_cmp_pos[j], nsa_cmp_w1[j], nsa_cmp_w2[j], nsa_w_out[j])
        h = h + rmsnorm(m, norm_g[layer, 1])
        u = rmsnorm(h, norm_g[layer, 2])
        h = h + rmsnorm(_swiglu(u, ffn_w_gu[layer], ffn_w_down[layer]), norm_g[layer, 3])
    return h
```

```python
from contextlib import ExitStack
import numpy as np
import ml_dtypes
import concourse.bass as bass
import concourse.mybir as mybir


F32 = mybir.dt.float32
BF16 = mybir.dt.bfloat16
ALU = mybir.AluOpType
AF = mybir.ActivationFunctionType
AX = mybir.AxisListType

EPOCH = 16000
NDS = 8
COMPUTE = ('pe', 'act', 'dve', 'pool')
QUEUES = ('sp', 'actq', 'poolq')
Q2ENG = {'sp': 'sp', 'actq': 'act', 'poolq': 'pool'}


class Unit:
    __slots__ = ('name', 'w', 'r')

    def __init__(self, name):
        self.name = name
        self.w = None
        self.r = []


class Buf:
    def __init__(self, t, name, nunits=1):
        self.t = t
        self.name = name
        self.units = [Unit(f"{name}.{i}") for i in range(nunits)]

    @property
    def u(self):
        return self.units[0]

    def __getitem__(self, k):
        return self.t[k]


class Prog:
    def __init__(self, nc):
        self.nc = nc
        self.streams = {e: [] for e in ('pe', 'act', 'dve', 'pool', 'sp')}
        self.ncomp = {e: 0 for e in COMPUTE}
        self.ndma = {q: 0 for q in QUEUES}
        self.waited = {e: {} for e in ('pe', 'act', 'dve', 'pool', 'sp')}
        self.semkeys = {}
        self.sb_off = 16512
        self.n_ops = 0
        self.out_tokens = []
        self.same_engine_sync = True
        self.barrier_toks = {}

    def sbuf(self, name, shape, dtype, nunits=1):
        esz = 4 if dtype in (F32, mybir.dt.int32, mybir.dt.uint32) else 2
        per_part = int(np.prod(shape[1:])) * esz
        off = (self.sb_off + 31) // 32 * 32
        t = self.nc.alloc_sbuf_tensor_at(name, list(shape), dtype, offset=off)
        self.sb_off = off + per_part
        assert self.sb_off <= 229344, f"SBUF overflow at {name}: {self.sb_off}"
        return Buf(t, name, nunits)

    def sbuf_mark(self):
        return self.sb_off

    def sbuf_reset(self, mark):
        self.sb_off = mark

    def psum(self, name, shape, dtype=F32, nunits=1):
        t = self.nc.alloc_psum_tensor(name, list(shape), dtype)
        return Buf(t, name, nunits)

    def _sem(self, key):
        if key not in self.semkeys:
            self.semkeys[key] = None
        return key

    def _deps(self, reads, writes):
        toks = []
        for u in reads:
            if u.w is not None:
                toks.append(u.w)
        for u in writes:
            if u.w is not None:
                toks.append(u.w)
            toks.extend(u.r)
        return toks

    def _commit(self, tok, reads, writes):
        for u in reads:
            u.r.append(tok)
        for u in writes:
            u.w = tok
            u.r = []

    def _waits_for(self, stream, toks, self_eng=None):
        need = {}
        for (key, val) in toks:
            if key[0] == 'eng' and key[1] == self_eng:
                if self_eng == 'pe' or not self.same_engine_sync:
                    continue
            if self.waited[stream].get(key, 0) >= val:
                continue
            if need.get(key, 0) < val:
                need[key] = val
        for key, val in need.items():
            self.waited[stream][key] = val
        return list(need.items())

    def op(self, eng, fn, reads=(), writes=()):
        assert eng in COMPUTE
        toks = self._deps(reads, writes) + self.barrier_toks.pop(eng, [])
        waits = self._waits_for(eng, toks, self_eng=eng)
        i = self.ncomp[eng]
        self.ncomp[eng] += 1
        key = self._sem(('eng', eng, i // EPOCH))
        tok = (key, i % EPOCH + 1)
        self.streams[eng].append((waits, fn, (key, 1)))
        self._commit(tok, reads, writes)
        self.n_ops += 1
        return tok

    def dma(self, q, out, in_, reads=(), writes=(), is_output=False, **kw):
        stream = Q2ENG[q]
        toks = self._deps(reads, writes) + self.barrier_toks.pop(stream, [])
        k = self.ndma[q]
        self.ndma[q] += 1
        key = self._sem(('dma', q, k % NDS))
        val = 16 * (k // NDS + 1)
        if k >= NDS:
            toks = toks + [(key, val - 16)]
        waits = self._waits_for(stream, toks, self_eng=None)
        tok = (key, val)

        def fn(e, out=out, in_=in_, kw=kw):
            return e.dma_start(out=out, in_=in_, **kw)
        self.streams[stream].append((waits, fn, (key, 16)))
        self._commit(tok, reads, writes)
        if is_output:
            self.out_tokens.append(tok)
        self.n_ops += 1
        return tok

    def barrier(self):
        toks = []
        for e in COMPUTE:
            n = self.ncomp[e]
            if n > 0:
                toks.append((('eng', e, (n - 1) // EPOCH), (n - 1) % EPOCH + 1))
        for q in QUEUES:
            k = self.ndma[q]
            for s in range(min(NDS, k)):
                cnt = (k - s + NDS - 1) // NDS
                toks.append((('dma', q, s), 16 * cnt))
        for st in self.streams:
            self.barrier_toks[st] = self.barrier_toks.get(st, []) + toks

    def emit(self):
        nc = self.nc
        fin = self._waits_for('sp', self.out_tokens)
        with ExitStack() as es:
            sems = {}
            for key in self.semkeys:
                nm = "s_" + "_".join(str(x) for x in key)
                sems[key] = es.enter_context(nc.semaphore(nm))
            block = es.enter_context(nc.Block())
            streams = self.streams

            def run(e, name):
                for waits, fn, inc in streams[name]:
                    for key, val in waits:
                        e.wait_ge(sems[key], val)
                    ins = fn(e)
                    if inc is not None:
                        ins.then_inc(sems[inc[0]], inc[1])

            @block.tensor
            def _(e):
                run(e, 'pe')

            @block.scalar
            def _(e):
                run(e, 'act')

            @block.vector
            def _(e):
                run(e, 'dve')

            @block.gpsimd
            def _(e):
                run(e, 'pool')

            @block.sync
            def _(e):
                run(e, 'sp')
                for key, val in fin:
                    e.wait_ge(sems[key], val)
        return nc


def _units(x):
    out = []
    for b in x:
        if isinstance(b, Buf):
            out.extend(b.units)
        elif isinstance(b, Unit):
            out.append(b)
        else:
            out.extend(_units(b))
    return out


class Ops:
    def __init__(self, P):
        self.P = P

    def mm(self, out, lhsT, rhs, start, stop, r, w):
        return self.P.op('pe', lambda e: e.matmul(out=out, lhsT=lhsT, rhs=rhs, start=start, stop=stop),
                         reads=_units(r), writes=_units(w))

    def tr(self, out, in_, ident, r, w):
        return self.P.op('pe', lambda e: e.transpose(out=out, in_=in_, identity=ident),
                         reads=_units(r), writes=_units(w))

    def act(self, out, in_, func, r, w, bias=None, scale=None, accum_out=None, eng='act'):
        kw = {}
        if bias is not None:
            kw['bias'] = bias
        if scale is not None:
            kw['scale'] = scale
        if accum_out is not None:
            kw['accum_out'] = accum_out
        return self.P.op('act', lambda e: e.activation(out=out, in_=in_, func=func, **kw),
                         reads=_units(r), writes=_units(w))

    def ts(self, eng, out, in0, s1, s2, op0, op1, r, w, accum_out=None):
        kw = {}
        if op1 is not None:
            kw['op1'] = op1
        if accum_out is not None:
            kw['accum_out'] = accum_out
        return self.P.op(eng, lambda e: e.tensor_scalar(out=out, in0=in0, scalar1=s1, scalar2=s2, op0=op0, **kw),
                         reads=_units(r), writes=_units(w))

    def tt(self, eng, out, in0, in1, op, r, w):
        return self.P.op(eng, lambda e: e.tensor_tensor(out=out, in0=in0, in1=in1, op=op),
                         reads=_units(r), writes=_units(w))

    def stt(self, eng, out, in0, scalar, in1, op0, op1, r, w):
        return self.P.op(eng, lambda e: e.scalar_tensor_tensor(out=out, in0=in0, scalar=scalar, in1=in1,
                                                               op0=op0, op1=op1),
                         reads=_units(r), writes=_units(w))

    def copy(self, eng, out, in_, r, w):
        if eng == 'act':
            return self.P.op('act', lambda e: e.copy(out=out, in_=in_), reads=_units(r), writes=_units(w))
        return self.P.op(eng, lambda e: e.tensor_copy(out=out, in_=in_), reads=_units(r), writes=_units(w))

    def memset(self, eng, out, val, w):
        return self.P.op(eng, lambda e: e.memset(out, val), reads=[], writes=_units(w))

    def dma(self, q, out, in_, r, w, **kw):
        return self.P.dma(q, out, in_, reads=_units(r), writes=_units(w), **kw)


D = 2048
DFF = 5632
NFF = DFF // 128
EPS = 1e-6


def alloc_common(P):
    C = {}
    C['pb'] = [P.psum(f"pb{i}", [128, 512], F32) for i in range(8)]
    C['ident'] = P.sbuf("ident_bf", [128, 128], BF16)
    C['identf'] = P.sbuf("ident_f", [128, 128], F32)
    C['gT0'] = P.sbuf("gT0", [128, 16], F32)
    return C


def load_common(P, O, C, cd):
    O.dma('sp', C['ident'][:], cd['ident_bf'][:], [cd['ident_bf']], [C['ident']])
    O.dma('sp', C['identf'][:], cd['ident_f'][:], [cd['ident_f']], [C['identf']])


def rstd_from_ssq(O, ssq_ap, out_ap, n, r, w):
    O.act(out_ap, ssq_ap, AF.Ln, r, w, scale=1.0 / n, bias=EPS)
    O.act(out_ap, out_ap, AF.Exp, w, w, scale=-0.5)


def build_F(P, O, C, io):
    pb = C['pb']
    ident = C['ident']
    mark = P.sbuf_mark()
    h = P.sbuf("F_h", [128, 8, D], F32, nunits=8)
    gb3 = P.sbuf("F_gb3", [128, D], F32)
    g2T = P.sbuf("F_g2T", [128, 16], F32)
    st = P.sbuf("F_st", [128, 16], F32)
    junk = P.sbuf("F_junk", [128, D], BF16)
    mark2 = P.sbuf_mark()
    gb1 = P.sbuf("F_gb1", [128, D], F32)

    for t in range(8):
        O.dma('sp', h[:, t, :], io['h_in'][t * 128:(t + 1) * 128, :], [io['h_in']], [h.units[t]])
    O.dma('sp', gb1[:], io['g'][0:1, :].to_broadcast([128, D]), [io['g']], [gb1])
    O.dma('sp', gb3[:], io['g'][2:3, :].to_broadcast([128, D]), [io['g']], [gb3])
    O.dma('sp', g2T[:], io['g'][1, :].rearrange("(c p) -> p c", p=128), [io['g']], [g2T],
          allow_slow_non_contiguous=True)

    wo = P.sbuf("F_wo", [128, 16, D], BF16, nunits=4)
    oT = P.sbuf("F_oT", [128, 16, 1024], BF16)
    O.dma('sp', oT[:], io['oT'].t.rearrange("(c p) t -> p c t", p=128), [io['oT']], [oT])
    for q in range(4):
        O.dma('poolq', wo[:, 4 * q:4 * q + 4, :],
              io['w_out'].t[512 * q:512 * (q + 1), :].rearrange("(c p) n -> p c n", p=128),
              [io['w_out']], [wo.units[q]])
    tmp = P.sbuf("F_tmp", [128, 512], F32)
    for t in range(8):
        banks = pb[0:4] if t % 2 == 0 else pb[4:8]
        for cb in range(4):
            for k in range(16):
                O.mm(banks[cb][:], oT[:, k, t * 128:(t + 1) * 128], wo[:, k, cb * 512:(cb + 1) * 512],
                     k == 0, k == 15, [oT, wo.units[k // 4]], [banks[cb]])
        for cb in range(4):
            O.act(junk[:, 0:512], banks[cb][:], AF.Square, [banks[cb]], [junk, st], accum_out=st[:, cb:cb + 1])
        O.P.op('dve', lambda e: e.reduce_sum(out=st[:, 4:5], in_=st[:, 0:4], axis=AX.X),
               reads=[st.u], writes=[st.u])
        rstd_from_ssq(O, st[:, 4:5], st[:, 5:6], D, [st], [st])
        for cb in range(4):
            O.stt('dve', tmp[:], banks[cb][:], st[:, 5:6], gb1[:, cb * 512:(cb + 1) * 512], ALU.mult, ALU.mult,
                  [banks[cb], st, gb1], [tmp])
            O.tt('dve', h[:, t, cb * 512:(cb + 1) * 512], h[:, t, cb * 512:(cb + 1) * 512], tmp[:], ALU.add,
                 [tmp, h.units[t]], [h.units[t]])

    P.barrier()
    P.sbuf_reset(mark2)
    uT = P.sbuf("F_uT", [128, 16, 512], BF16, nunits=4)
    wgu = [P.sbuf(f"F_wgu{i}", [128, 16, 2, 128], BF16) for i in range(3)]
    mark3 = P.sbuf_mark()
    actT = P.sbuf("F_actT", [128, NFF, 512], BF16, nunits=NFF)
    f = P.sbuf("F_f", [128, 4, D], F32, nunits=4)
    xn = P.sbuf("F_xn", [128, D], BF16)
    sg = [P.sbuf(f"F_sg{i}", [128, 512], BF16) for i in range(2)]
    save = P.sbuf_mark()
    P.sbuf_reset(mark2)
    wd = [P.sbuf(f"F_wd{i}", [128, 11, 512], BF16) for i in range(3)]
    assert P.sbuf_mark() <= mark3
    P.sbuf_reset(save)
    pbT = [pb[i].t.ap().bitcast(BF16) for i in range(8)]

    w_gu = io['w_gu'].t
    w_down = io['w_down'].t
    for grp in range(2):
        for tt in range(4):
            t = grp * 4 + tt
            O.act(junk[:], h[:, t, :], AF.Square, [h.units[t]], [junk, st], accum_out=st[:, 6:7])
            rstd_from_ssq(O, st[:, 6:7], st[:, 7:8], D, [st], [st])
            O.ts('dve', xn[:], h[:, t, :], st[:, 7:8], None, ALU.mult, None, [h.units[t], st], [xn])
            for half in range(2):
                bk = pb[2 * (tt % 2) + half]
                bkT = pbT[2 * (tt % 2) + half]
                for c in range(8):
                    ch = half * 8 + c
                    O.tr(bkT[:, c * 128:(c + 1) * 128], xn[:, ch * 128:(ch + 1) * 128], ident[:],
                         [xn, ident], [bk])
                O.tt('dve', uT[:, half * 8:half * 8 + 8, tt * 128:(tt + 1) * 128],
                     bkT.rearrange("p (c t) -> p c t", c=8),
                     g2T[:, half * 8:half * 8 + 8].unsqueeze(2).to_broadcast([128, 8, 128]), ALU.mult,
                     [bk, g2T], [uT.units[tt]])
        for j in range(NFF):
            wb = wgu[j % 3]
            O.dma('poolq', wb[:, :, 0, :], w_gu[:, 128 * j:128 * (j + 1)].rearrange("(c p) n -> p c n", p=128),
                  [io['w_gu']], [wb])
            O.dma('poolq', wb[:, :, 1, :],
                  w_gu[:, DFF + 128 * j:DFF + 128 * (j + 1)].rearrange("(c p) n -> p c n", p=128),
                  [io['w_gu']], [wb])
            pg = pb[(j % 2) * 2]
            pu = pb[(j % 2) * 2 + 1]
            for k in range(16):
                O.mm(pg[:], wb[:, k, 0, :], uT[:, k, :], k == 0, k == 15, [wb, uT], [pg])
            for k in range(16):
                O.mm(pu[:], wb[:, k, 1, :], uT[:, k, :], k == 0, k == 15, [wb, uT], [pu])
            sgb = sg[j % 2]
            O.act(sgb[:], pg[:], AF.Silu, [pg], [sgb])
            O.tt('dve', actT[:, j, :], pu[:], sgb[:], ALU.mult, [pu, sgb], [actT.units[j]])
        P.barrier()
        ip = 0
        for cb in range(4):
            banks = pb[0:4] if cb % 2 == 0 else pb[4:8]
            for pc in range(4):
                wdb = wd[ip % 3]
                ip += 1
                O.dma('poolq', wdb[:],
                      w_down[pc * 1408:(pc + 1) * 1408, cb * 512:(cb + 1) * 512].rearrange("(c p) n -> p c n", p=128),
                      [io['w_down']], [wdb])
                for tt in range(4):
                    for kk in range(11):
                        j = pc * 11 + kk
                        O.mm(banks[tt][:], actT[:, j, tt * 128:(tt + 1) * 128], wdb[:, kk, :],
                             j == 0, j == NFF - 1, [actT.units[j], wdb], [banks[tt]])
            for tt in range(4):
                O.copy('act', f[:, tt, cb * 512:(cb + 1) * 512], banks[tt][:], [banks[tt]], [f.units[tt]])
        for tt in range(4):
            t = grp * 4 + tt
            O.act(junk[:], f[:, tt, :], AF.Square, [f.units[tt]], [junk, st], accum_out=st[:, 8:9])
            rstd_from_ssq(O, st[:, 8:9], st[:, 9:10], D, [st], [st])
            O.stt('dve', f[:, tt, :], f[:, tt, :], st[:, 9:10], gb3[:], ALU.mult, ALU.mult,
                  [f.units[tt], st, gb3], [f.units[tt]])
            O.tt('dve', h[:, t, :], h[:, t, :], f[:, tt, :], ALU.add, [f.units[tt], h.units[t]], [h.units[t]])
            O.dma('sp', io['h_out'].t[t * 128:(t + 1) * 128, :], h[:, t, :], [h.units[t]], [io['h_out']],
                  is_output=io.get('final', False))
        P.barrier()
    P.sbuf_reset(mark)


T = 2048
NT = T // 128
STOP = [None]


class _Stop(Exception):
    pass


def ck(n):
    if STOP[0] == n:
        raise _Stop()
NEG = -1.0e5


class Banks:
    def __init__(self, pb):
        self.pb = pb
        self.i = 0

    def get(self):
        b = self.pb[self.i % len(self.pb)]
        self.i += 1
        return b


def compute_uT(P, O, C, h_full, g_row, uT, scratch, st):
    pb = C['pb']
    ident = C['ident']
    gT = C['gT0']
    O.dma('sp', gT[:], g_row.rearrange("(c p) -> p c", p=128), [], [gT], allow_slow_non_contiguous=True)
    pbT = [pb[i].t.ap().bitcast(BF16) for i in range(8)]
    xnb = scratch[2]
    xn = xnb.t.ap().bitcast(BF16)
    for t in range(NT):
        hs = scratch[t % 2]
        O.dma('sp', hs[:], h_full.t[t * 128:(t + 1) * 128, :], [h_full], [hs])
        O.act(xn[:, 2048:4096], hs[:], AF.Square, [hs], [xnb, st], accum_out=st[:, 0:1])
        rstd_from_ssq(O, st[:, 0:1], st[:, 1:2], D, [st], [st])
        O.ts('dve', xn[:, 0:2048], hs[:], st[:, 1:2], None, ALU.mult, None, [hs, st, xnb], [xnb])
        for half in range(2):
            bi = (2 * t + half) % 8
            for c in range(8):
                ch = half * 8 + c
                O.tr(pbT[bi][:, c * 128:(c + 1) * 128], xn[:, ch * 128:(ch + 1) * 128], ident[:], [xnb, ident], [pb[bi]])
            O.tt('dve', uT[:, half * 8:half * 8 + 8, t * 128:(t + 1) * 128],
                 pbT[bi].rearrange("p (c t) -> p c t", c=8),
                 gT[:, half * 8:half * 8 + 8].unsqueeze(2).to_broadcast([128, 8, 128]), ALU.mult,
                 [pb[bi], gT], [uT.units[t // 4]])


class WRing:
    def __init__(self, P, O, n, name):
        self.slots = [P.sbuf(f"{name}{i}", [128, 16, 128], BF16) for i in range(n)]
        self.i = 0
        self.O = O

    def load(self, wbuf, col0, ncols=128):
        s = self.slots[self.i % len(self.slots)]
        self.i += 1
        self.O.dma('poolq', s[:, :, 0:ncols], wbuf.t[:, col0:col0 + ncols].rearrange("(c p) n -> p c n", p=128),
                   [wbuf], [s])
        return s


def proj_fm(O, bk, uT, ws, evac):
    for g in range(4):
        b = bk.get()
        for k in range(16):
            O.mm(b[:], ws[:, k, :], uT[:, k, g * 512:(g + 1) * 512], k == 0, k == 15, [ws, uT.units[g]], [b])
        evac(g, b)


def proj_tm(O, bk, uT, ws, ncols, evac):
    for q in range(4):
        b = bk.get()
        for j in range(4):
            t = q * 4 + j
            for k in range(16):
                O.mm(b[:, j * 128:j * 128 + ncols], uT[:, k, t * 128:(t + 1) * 128], ws[:, k, 0:ncols],
                     k == 0, k == 15, [ws, uT.units[q]], [b])
        evac(q, b)


def build_ME(P, O, C, io, layer):
    try:
        _build_ME(P, O, C, io, layer)
    except _Stop:
        dummy = P.sbuf("ME_dummy", [128, T], BF16)
        O.memset('dve', dummy[:], 1.0, [dummy])
        O.dma('sp', io['oT_out'].t[0:128, :], dummy[:], [dummy], [io['oT_out']], is_output=True)


def _build_ME(P, O, C, io, layer):
    pb = C['pb']
    ident, identf = C['ident'], C['identf']
    bk = Banks(pb)
    mark = P.sbuf_mark()
    st = P.sbuf("ME_st", [128, 16], F32)
    uT = P.sbuf("ME_uT", [128, 16, T], BF16, nunits=4)
    scr = [P.sbuf(f"ME_s{i}", [128, T], F32) for i in range(4)]
    ring = WRing(P, O, 2, "ME_w")
    pers = [[P.sbuf(f"ME_p{h}_{i}", [128, T], BF16) for i in range(8)] for h in range(2)]
    oTs = [P.sbuf(f"ME_oT{h}", [128, T], BF16) for h in range(2)]
    S32 = [P.sbuf(f"ME_S32_{h}", [128, 128], F32) for h in range(2)]
    Sbf = [P.sbuf(f"ME_Sbf_{h}", [128, 128], BF16) for h in range(2)]
    cst = C['me_const']
    lb = P.sbuf("ME_lb", [128, 8], F32)
    cw = P.sbuf("ME_cw", [128, 12, 4], F32)
    hgT = P.sbuf("ME_hgT", [128, 2], F32)
    prm = P.sbuf("ME_prm", [128, 8], F32)
    zs = P.sbuf("ME_zs", [128, NT, 8], F32)
    tok = P.sbuf("ME_tok", [128, NT, 4, 12], F32)
    sc4 = P.sbuf("ME_sc4", [128, NT, 4, 4], F32)
    egr = P.sbuf("ME_egr", [128, NT, 2, 4], F32)
    tmpA = [P.sbuf(f"ME_tA{i}", [128, 128], F32) for i in range(4)]
    tmpB = [P.sbuf(f"ME_tB{i}", [128, 128], BF16) for i in range(16)]
    vnew = [[P.sbuf(f"ME_vn{h}_{c}", [128, 128], BF16) for c in range(2)] for h in range(2)]
    u32 = [P.sbuf(f"ME_u32_{h}", [128, 128], F32) for h in range(2)]

    w_in = io['w_in']
    O.dma('sp', cw[:], io['conv_w'].t, [io['conv_w']], [cw])
    O.dma('sp', hgT[:, 0:1], io['hg'].t.rearrange("(p o) -> p o", o=1), [io['hg']], [hgT])
    O.dma('sp', hgT[:, 1:2], io['gg'].t.rearrange("(p o) -> p o", o=1), [io['gg']], [hgT])
    O.dma('sp', prm[:, 0:4], io['a_log'].t.rearrange("(o n) -> o n", o=1).to_broadcast([128, 4]), [io['a_log']], [prm])
    O.dma('sp', prm[:, 4:8], io['dt_bias'].t.rearrange("(o n) -> o n", o=1).to_broadcast([128, 4]), [io['dt_bias']], [prm])
    O.act(prm[:, 0:4], prm[:, 0:4], AF.Exp, [prm], [prm])
    O.ts('dve', prm[:, 0:4], prm[:, 0:4], -1.0, None, ALU.mult, None, [prm], [prm])
    if layer // 2 == 1:
        lbl = P.sbuf("ME_lbl", [128, 2, 4], F32)
        O.dma('sp', lbl[:], io['lbl'].t, [io['lbl']], [lbl])
        O.tt('dve', lb[:, 0:4], lbl[:, 1, :], lbl[:, 0, :], ALU.subtract, [lbl], [lb])
        O.act(lb[:, 0:4], lb[:, 0:4], AF.Sigmoid, [lb], [lb])
        O.ts('dve', lb[:, 4:8], lb[:, 0:4], -1.0, 1.0, ALU.mult, ALU.add, [lb], [lb])
    for h in range(2):
        for c in range(2):
            O.memset('pool', vnew[h][c][:], 0.0, [vnew[h][c]])
    ck(0)
    compute_uT(P, O, C, io['h_full'], io['g0'], uT, scr, st)
    ck(1)

    wsm = ring.load(w_in, 4096, 8)

    def ev_zs(q, b):
        O.copy('act', zs[:, 4 * q:4 * q + 4, :], b[:].rearrange("p (j c) -> p j c", j=4)[:, :, 0:8], [b], [zs])
    proj_tm(O, bk, uT, wsm, 8, ev_zs)
    tk = lambda i: tok[:, :, :, i]
    bc4 = lambda ap: ap.unsqueeze(1).to_broadcast([128, NT, 4])
    O.act(tk(0), zs[:, :, 4:8], AF.Sigmoid, [zs], [tok])
    O.act(tk(1), tk(0), AF.Ln, [tok], [tok])
    O.tt('dve', tk(10), zs[:, :, 0:4], bc4(prm[:, 4:8]), ALU.add, [zs, prm], [tok])
    O.stt('dve', tk(11), tk(10), -1.0, tk(10), ALU.mult, ALU.max, [tok], [tok])
    O.act(tk(11), tk(11), AF.Exp, [tok], [tok], scale=-1.0)
    O.act(tk(11), tk(11), AF.Ln, [tok], [tok], bias=1.0)
    O.stt('dve', tk(10), tk(10), 0.0, tk(11), ALU.max, ALU.add, [tok], [tok])
    O.tt('dve', tk(2), tk(10), bc4(prm[:, 0:4]), ALU.mult, [tok, prm], [tok])
    for t in range(NT):
        b = bk.get()
        O.mm(b[:, 0:4], cst['tri'][:], tok[:, t, :, 2], True, True, [cst['tri'], tok], [b])
        O.mm(b[:, 4:8], cst['blk'][:], tok[:, t, :, 2], True, True, [cst['blk'], tok], [b])
        O.mm(b[:, 8:12], cst['ch0'][:], tok[:, t, :, 2], True, True, [cst['ch0'], tok], [b])
        O.mm(b[:, 12:16], cst['ch1'][:], tok[:, t, :, 2], True, True, [cst['ch1'], tok], [b])
        O.copy('dve', tok[:, t, :, 3:5].rearrange("p h s -> p s h"), b[:, 0:8].rearrange("p (s h) -> p s h", s=2), [b], [tok])
        O.act(egr[:, t, :, :], b[:, 8:16].rearrange("p (c h) -> p c h", c=2), AF.Exp, [b], [egr])

    ck(2)
    def hgrn_prep(hl, hi):
        qtl, ktl, qh, khm0, khm1, vtok, sgg, _ = pers[hl]
        vt = vtok[:].rearrange("p (t e) -> p t e", e=128)
        s0, s1, s2, s3 = scr
        ws = ring.load(w_in, 0 + hi * 128)
        proj_fm(O, bk, uT, ws, lambda g, b: O.copy('act', s0[:, g * 512:(g + 1) * 512], b[:], [b], [s0]))
        ws = ring.load(w_in, 512 + hi * 128)
        proj_fm(O, bk, uT, ws, lambda g, b: O.act(s1[:, g * 512:(g + 1) * 512], b[:], AF.Sigmoid, [b], [s1]))
        ws = ring.load(w_in, 1024 + hi * 128)
        proj_tm(O, bk, uT, ws, 128, lambda q, b: O.copy('act', vt[:, 4 * q:4 * q + 4, :], b[:].rearrange("p (j c) -> p j c", j=4), [b], [vtok]))
        ws = ring.load(w_in, 1536 + hi * 128)

        def ev_g(g, b):
            O.act(s2[:, g * 512:(g + 1) * 512], b[:], AF.Silu, [b], [s2])
        proj_fm(O, bk, uT, ws, ev_g)
        O.ts('dve', sgg[:], s2[:], hgT[:, 0:1], None, ALU.mult, None, [s2, hgT], [sgg])
        if layer // 2 == 1:
            O.ts('dve', s1[:], s1[:], lb[:, 4 + hi:5 + hi], lb[:, hi:hi + 1], ALU.mult, ALU.add, [s1, lb], [s1])
        O.act(s2[:], s1[:], AF.Ln, [s1], [s2])
        O.ts('dve', s1[:], s1[:], -1.0, 1.0, ALU.mult, ALU.add, [s1], [s1])
        P.op('dve', lambda e: e.tensor_tensor_scan(out=s3[:], data0=cst['rst'][:], data1=s2[:], initial=0.0,
                                                   op0=ALU.mult, op1=ALU.add),
             reads=[cst['rst'].u, s2.u], writes=[s3.u])
        b3 = s3[:].rearrange("p (n c) -> p n c", c=64)
        s2v = s2[:].rearrange("p (n c) -> p n c", c=64)
        ebend = pers[hl][7]
        ebv = ebend.t.ap().bitcast(F32)
        O.act(ebv[:, 0:32], b3[:, :, 63], AF.Exp, [s3], [ebend])
        O.tt('dve', s2v, b3, b3[:, :, 32:33].to_broadcast([128, 32, 64]), ALU.subtract, [s3], [s2])
        O.act(s2[:], s2[:], AF.Exp, [s2], [s2])
        O.tt('dve', qtl[:], s0[:], s2[:], ALU.mult, [s0, s2], [qtl])
        O.P.op('dve', lambda e: e.reciprocal(out=s2[:], in_=s2[:]), reads=[s2.u], writes=[s2.u])
        O.tt('dve', ktl[:], s1[:], s2[:], ALU.mult, [s1, s2], [ktl])
        O.act(s2[:], s3[:], AF.Exp, [s3], [s2])
        O.tt('dve', qh[:], s0[:], s2[:], ALU.mult, [s0, s2], [qh])
        O.tt('dve', s2v, b3[:, :, 63:64].to_broadcast([128, 32, 64]), b3, ALU.subtract, [s3], [s2])
        O.act(s2[:], s2[:], AF.Exp, [s2], [s2])
        s0b = s0.t.ap().bitcast(BF16)
        O.tt('dve', s0b[:, 0:T], s1[:], s2[:], ALU.mult, [s1, s2, s0], [s0])
        pbT = [pb[i].t.ap().bitcast(BF16) for i in range(8)]
        k0 = khm0[:].rearrange("p (t d) -> p t d", d=128)
        k1 = khm1[:].rearrange("p (t d) -> p t d", d=128)
        for half in range(2):
            b = bk.get()
            bT = b.t.ap().bitcast(BF16)
            for c in range(8):
                t = half * 8 + c
                O.tr(bT[:, c * 128:(c + 1) * 128], s0b[:, t * 128:(t + 1) * 128], ident[:], [s0, ident], [b])
            O.ts('dve', k0[:, half * 8:half * 8 + 8, :], bT.rearrange("p (t d) -> p t d", d=128), cst['m01'][:, 0:1], None,
                 ALU.mult, None, [b, cst['m01']], [khm0])
            O.ts('dve', k1[:, half * 8:half * 8 + 8, :], bT.rearrange("p (t d) -> p t d", d=128), cst['m01'][:, 1:2], None,
                 ALU.mult, None, [b, cst['m01']], [khm1])
        O.memset('dve', S32[hl][:], 0.0, [S32[hl]])
        O.memset('dve', Sbf[hl][:], 0.0, [Sbf[hl]])

    def post(hl, t, pO, gcol):
        sgg = pers[hl][6]
        sq = tmpB[8 + hl * 4]
        O.act(sq[:], pO[:, 0:128], AF.Square, [pO], [sq])
        pN = bk.get()
        O.mm(pN[:, 0:128], cst['onesb'][:], sq[:], True, True, [cst['onesb'], sq], [pN])
        rs = tmpA[2 + hl]
        O.act(rs[:], pN[:, 0:128], AF.Ln, [pN], [rs], scale=1.0 / 128, bias=EPS)
        O.act(rs[:], rs[:], AF.Exp, [rs], [rs], scale=-0.5)
        O.tt('dve', rs[:], pO[:, 0:128], rs[:], ALU.mult, [pO, rs], [rs])
        O.tt('dve', oTs[hl][:, t * 128:(t + 1) * 128], rs[:], sgg[:, t * 128:(t + 1) * 128], ALU.mult, [rs, sgg], [oTs[hl]])

    def hgrn_tile(hl, t):
        qtl, ktl, qh, khm0, khm1, vtok, sgg, ebend = pers[hl]
        ebv = ebend.t.ap().bitcast(F32)
        vt = vtok[:].rearrange("p (t e) -> p t e", e=128)
        khm = [khm0[:].rearrange("p (t d) -> p t d", d=128), khm1[:].rearrange("p (t d) -> p t d", d=128)]
        ts_ = slice(t * 128, (t + 1) * 128)
        pI = bk.get()
        O.mm(pI[:, 0:128], ktl[:, ts_], qtl[:, ts_], True, True, [ktl, qtl], [pI])
        isb = tmpB[hl * 4]
        O.tt('dve', isb[:], pI[:, 0:128], cst['maskT'][:], ALU.mult, [pI, cst['maskT']], [isb])
        pO = bk.get()
        O.mm(pO[:, 0:128], vt[:, t, :], isb[:], True, False, [vtok, isb], [pO])
        for cc in range(2):
            n = 2 * t + cc
            O.mm(pO[:, cc * 64:(cc + 1) * 64], Sbf[hl][:], qh[:, t * 128 + cc * 64:t * 128 + (cc + 1) * 64], False, cc == 1,
                 [Sbf[hl], qh], [pO])
            pS = bk.get()
            O.mm(pS[:, 0:128], khm[cc][:, t, :], vt[:, t, :], True, True, [khm0, khm1, vtok], [pS])
            O.stt('dve', S32[hl][:], S32[hl][:], ebv[:, n:n + 1], pS[:, 0:128], ALU.mult, ALU.add, [S32[hl], ebend, pS], [S32[hl]])
            O.copy('act', Sbf[hl][:], S32[hl][:], [S32[hl]], [Sbf[hl]])
        post(hl, t, pO, 0)

    def gdn_prep(hl, hi):
        kcT, qcT, vb, kbg, kdm0, kdm1, sgg, qgt = pers[hl]
        s0, s1, s2, s3 = scr
        tkh = lambda i: tok[:, :, hi, i]
        raw = {}
        for nm, col, dst in (('q', 2048, s0), ('k', 2560, s1), ('v', 3072, s2)):
            ws = ring.load(w_in, col + hi * 128)
            proj_fm(O, bk, uT, ws, lambda g, b, dst=dst: O.copy('act', dst[:, g * 512:(g + 1) * 512], b[:], [b], [dst]))
        ws = ring.load(w_in, 3584 + hi * 128)
        proj_fm(O, bk, uT, ws, lambda g, b: O.act(s3[:, g * 512:(g + 1) * 512], b[:], AF.Silu, [b], [s3]))
        O.ts('dve', sgg[:], s3[:], hgT[:, 1:2], None, ALU.mult, None, [s3, hgT], [sgg])
        s3b = s3.t.ap().bitcast(BF16)
        for ci, (src, dstap, dstbuf) in enumerate(((s0, qcT[:], qcT), (s1, kcT[:], kcT), (s2, s3b[:, 0:T], s3))):
            widx = ci * 4 + hi
            acc = s3 if ci < 2 else s0
            O.ts('pool', acc[:], src[:], cw[:, widx, 3:4], None, ALU.mult, None, [src, cw], [acc])
            for j in range(3):
                sh = 3 - j
                O.stt('dve', acc[:, sh:T], src[:, 0:T - sh], cw[:, widx, j:j + 1], acc[:, sh:T], ALU.mult, ALU.add,
                      [src, cw, acc], [acc])
            O.act(dstap, acc[:], AF.Silu, [acc], [dstbuf])
        views = []
        for src_ap, srcbuf, dst in ((qcT[:], qcT, s0), (kcT[:], kcT, s1), (s3b[:, 0:T], s3, s2)):
            dv = dst.t.ap().bitcast(BF16)[:, 0:T].rearrange("p (t d) -> p t d", d=128)
            for half in range(2):
                b = bk.get()
                bT = b.t.ap().bitcast(BF16)
                for c in range(8):
                    t = half * 8 + c
                    O.tr(bT[:, c * 128:(c + 1) * 128], src_ap[:, t * 128:(t + 1) * 128], ident[:], [srcbuf, ident], [b])
                O.copy('act', dv[:, half * 8:half * 8 + 8, :], bT.rearrange("p (t d) -> p t d", d=128), [b], [dst])
            views.append(dv)
        qv, kv, vv = views
        jf = s3[:].rearrange("p (t d) -> p t d", d=128)
        for src, srcbuf, slot in ((qv, s0, 5), (kv, s1, 6)):
            O.tt('dve', jf, src, src, ALU.mult, [srcbuf], [s3])
            P.op('dve', lambda e, slot=slot: e.tensor_reduce(out=tkh(slot), in_=jf, axis=AX.X, op=ALU.add),
                 reads=[s3.u], writes=[tok.u])
            O.act(tkh(slot), tkh(slot), AF.Ln, [tok], [tok], bias=EPS)
            O.ts('dve', tkh(slot), tkh(slot), -0.5, None, ALU.mult, None, [tok], [tok])
        O.tt('dve', tkh(7), tkh(3), tkh(1), ALU.add, [tok], [tok])
        O.tt('dve', tkh(7), tkh(7), tkh(6), ALU.add, [tok], [tok])
        O.tt('dve', tkh(8), tkh(6), tkh(3), ALU.subtract, [tok], [tok])
        O.stt('dve', tkh(9), tkh(3), float(np.log(128 ** -0.5)), tkh(5), ALU.add, ALU.add, [tok], [tok])
        O.act(sc4[:, :, hi, 0], tkh(7), AF.Exp, [tok], [sc4])
        O.tt('dve', tkh(10), tkh(8), tkh(4), ALU.add, [tok], [tok])
        O.act(sc4[:, :, hi, 1], tkh(10), AF.Exp, [tok], [sc4])
        O.act(sc4[:, :, hi, 2], tkh(9), AF.Exp, [tok], [sc4])
        bct = lambda ap: ap.unsqueeze(2).to_broadcast([128, NT, 128])
        r3 = lambda buf: buf[:].rearrange("p (t d) -> p t d", d=128)
        O.tt('dve', r3(vb), vv, bct(tkh(0)), ALU.mult, [s2, tok], [vb])
        O.tt('dve', r3(kbg), kv, bct(sc4[:, :, hi, 0]), ALU.mult, [s1, sc4], [kbg])
        O.tt('dve', r3(qgt), qv, bct(sc4[:, :, hi, 2]), ALU.mult, [s0, sc4], [qgt])
        O.tt('dve', r3(kdm0), kv, bct(sc4[:, :, hi, 1]), ALU.mult, [s1, sc4], [kdm0])
        O.ts('dve', kdm1[:], kdm0[:], cst['m01'][:, 1:2], None, ALU.mult, None, [kdm0, cst['m01']], [kdm1])
        O.ts('dve', kdm0[:], kdm0[:], cst['m01'][:, 0:1], None, ALU.mult, None, [kdm0, cst['m01']], [kdm0])
        O.memset('dve', S32[hl][:], 0.0, [S32[hl]])
        O.memset('dve', Sbf[hl][:], 0.0, [Sbf[hl]])

    def gdn_tile(hl, hi, t):
        kcT, qcT, vb, kbg, kdm0, kdm1, sgg, qgt = pers[hl]
        r3 = lambda buf: buf[:].rearrange("p (t d) -> p t d", d=128)
        ts_ = slice(t * 128, (t + 1) * 128)
        tb = lambda i: tmpB[hl * 4 + i]
        X, XT, Y, Bq = tmpB[hl * 4 + 0], tmpB[hl * 4 + 1], tmpB[hl * 4 + 2], tmpB[hl * 4 + 3]
        dg = tmpA[hl]
        pG = bk.get()
        O.mm(pG[:, 0:128], kcT[:, ts_], kcT[:, ts_], True, True, [kcT], [pG])
        O.ts('dve', dg[:], identf[:], tok[:, t, hi, 8:9], None, ALU.mult, None, [identf, tok], [dg])
        pE = bk.get()
        O.mm(pE[:, 0:128], cst['onesf'][:], dg[:], True, False, [cst['onesf'], dg], [pE])
        O.mm(pE[:, 0:128], identf[:], cst['mstrict'][:], False, True, [identf, cst['mstrict']], [pE])
        E1 = tmpA[2 + hl]
        O.act(E1[:], pE[:, 0:128], AF.Exp, [pE, tok], [E1], bias=tok[:, t, hi, 7:8])
        O.stt('dve', X[:], pG[:, 0:128], -1.0, E1[:], ALU.mult, ALU.mult, [pG, E1], [X])
        ck(10)
        pT = bk.get()
        pTb = pT.t.ap().bitcast(BF16)
        O.tr(pTb[:, 0:128], X[:], ident[:], [X, ident], [pT])
        ck(101)
        O.copy('act', XT[:], pTb[:, 0:128], [pT], [XT])
        ck(102)
        O.tt('dve', Y[:], XT[:], ident[:], ALU.add, [XT, ident], [Y])
        ck(11)
        for lvl in range(5):
            p1 = bk.get()
            O.mm(p1[:, 0:128], XT[:], X[:], True, True, [XT, X], [p1])
            if lvl < 4:
                O.mm(p1[:, 128:256], X[:], XT[:], True, True, [XT, X], [p1])
            O.copy('act', X[:], p1[:, 0:128], [p1], [X])
            if lvl < 4:
                O.copy('act', XT[:], p1[:, 128:256], [p1], [XT])
            p2 = bk.get()
            O.mm(p2[:, 0:128], X[:], Y[:], True, True, [X, Y], [p2])
            O.tt('dve', Y[:], p2[:, 0:128], Y[:], ALU.add, [p2, Y], [Y])
        ck(12)
        p3 = bk.get()
        O.mm(p3[:, 0:128], Y[:], r3(vb)[:, t, :], True, True, [Y, vb], [p3])
        O.mm(p3[:, 128:256], r3(kbg)[:, t, :], Y[:], True, True, [Y, kbg], [p3])
        O.copy('act', u32[hl][:], p3[:, 0:128], [p3], [u32[hl]])
        wT = XT
        O.copy('act', wT[:], p3[:, 128:256], [p3], [wT])
        ck(13)
        p4 = bk.get()
        O.mm(p4[:, 0:128], kcT[:, ts_], qcT[:, ts_], True, True, [kcT, qcT], [p4])
        O.ts('dve', dg[:], identf[:], tok[:, t, hi, 9:10], None, ALU.mult, None, [identf, tok], [dg])
        O.mm(p4[:, 128:256], cst['onesf'][:], dg[:], True, False, [cst['onesf'], dg], [p4])
        O.mm(p4[:, 128:256], identf[:], cst['mincl'][:], False, True, [identf, cst['mincl']], [p4])
        O.act(E1[:], p4[:, 128:256], AF.Exp, [p4, tok], [E1], bias=tok[:, t, hi, 8:9])
        qkT = X
        O.tt('dve', qkT[:], p4[:, 0:128], E1[:], ALU.mult, [p4, E1], [qkT])
        ck(14)
        p5 = bk.get()
        p5b = p5.t.ap().bitcast(BF16)
        O.tr(p5b[:, 0:128], r3(qgt)[:, t, :], ident[:], [qgt, ident], [p5])
        qgT = Bq
        O.copy('act', qgT[:], p5b[:, 0:128], [p5], [qgT])
        ck(15)
        pO = bk.get()
        kdm = [r3(kdm0), r3(kdm1)]
        for cc in range(2):
            cs = slice(cc * 64, (cc + 1) * 64)
            p6 = bk.get()
            O.mm(p6[:, 0:128], wT[:], Sbf[hl][:], True, True, [wT, Sbf[hl]], [p6])
            vn = vnew[hl][cc]
            O.tt('dve', vn[:], u32[hl][:], p6[:, 0:128], ALU.subtract, [u32[hl], p6], [vn])
            O.mm(pO[:, cs], Sbf[hl][:], qgT[:, cs], True, False, [Sbf[hl], qgT], [pO])
            O.mm(pO[:, cs], vn[:], qkT[:, cs], False, True, [vn, qkT], [pO])
            O.mm(p6[:, 128:256], kdm[cc][:, t, :], vn[:], True, True, [kdm0, kdm1, vn], [p6])
            O.stt('dve', S32[hl][:], S32[hl][:], egr[:, t, cc, hi:hi + 1], p6[:, 128:256], ALU.mult, ALU.add,
                  [S32[hl], egr, p6], [S32[hl]])
            O.copy('act', Sbf[hl][:], S32[hl][:], [S32[hl]], [Sbf[hl]])
        ck(16)
        post(hl, t, pO, 1)

    oT_out = io['oT_out']
    for pair in range(2):
        for hl in range(2):
            hgrn_prep(hl, pair * 2 + hl)
        ck(3)
        for t in range(NT):
            for hl in range(2):
                hgrn_tile(hl, t)
        ck(4)
        for hl in range(2):
            hi = pair * 2 + hl
            O.dma('sp', oT_out.t[hi * 128:(hi + 1) * 128, :], oTs[hl][:], [oTs[hl]], [oT_out], is_output=io.get('final', False))
    for pair in range(2):
        for hl in range(2):
            gdn_prep(hl, pair * 2 + hl)
        ck(5)
        for t in range(NT):
            for hl in range(2):
                gdn_tile(hl, pair * 2 + hl, t)
        ck(6)
        for hl in range(2):
            hi = pair * 2 + hl
            O.dma('sp', oT_out.t[512 + hi * 128:512 + (hi + 1) * 128, :], oTs[hl][:], [oTs[hl]], [oT_out],
                  is_output=io.get('final', False))
    P.barrier()
    P.sbuf_reset(mark)


def build_MO(P, O, C, io):
    pb = C['pb']
    ident, identf = C['ident'], C['identf']
    cst = C['mo_const']
    bkr = Banks(pb[4:8])
    bk = Banks(pb)
    mark = P.sbuf_mark()
    st = P.sbuf("MO_st", [128, 16], F32)
    uT = P.sbuf("MO_uT", [128, 16, T], BF16, nunits=4)
    ring = WRing(P, O, 2, "MO_w")
    gates = P.sbuf("MO_gates", [128, NT, 24], F32)
    bias_c = P.sbuf("MO_bc", [128, 8, 16], F32)
    bias_s = P.sbuf("MO_bs", [128, 8, 16], F32)
    w1 = [P.sbuf(f"MO_w1_{i}", [128, 32, 128], BF16) for i in range(2)]
    w2 = [P.sbuf(f"MO_w2_{i}", [128, 128], BF16) for i in range(2)]
    posT = P.sbuf("MO_posT", [128, 2, 32], F32)
    posTb = P.sbuf("MO_posTb", [128, 2, 32], BF16)
    cvec = P.sbuf("MO_cvec", [128, 2], F32)
    qT = P.sbuf("MO_qT", [128, 4, T], BF16, nunits=4)
    kcT = P.sbuf("MO_kcT", [128, T], BF16)
    vcT = P.sbuf("MO_vcT", [128, T], BF16)
    ksT = P.sbuf("MO_ksT", [128, T], BF16)
    kwT = P.sbuf("MO_kwT", [128, T], BF16)
    vsa = P.sbuf("MO_vsa", [128, NT, 129], BF16)
    vwa = P.sbuf("MO_vwa", [128, NT, 129], BF16)
    h1 = [P.sbuf(f"MO_h1_{i}", [128, 128], BF16) for i in range(2)]
    kcmpT = P.sbuf("MO_kcmpT", [128, 128], BF16)
    vaug = P.sbuf("MO_vaug", [128, 161], BF16)
    oTs = [P.sbuf(f"MO_oT{h}", [128, T], BF16) for h in range(4)]
    PTc = P.sbuf("MO_PTc", [128, 4, 128], BF16)
    PTs = [P.sbuf(f"MO_PT{i}", [128, 4, 128], BF16) for i in range(6)]
    oacc = P.sbuf("MO_oacc", [128, 4, 128], F32)
    oab = P.sbuf("MO_oab", [128, 4, 128], BF16)
    sm = P.sbuf("MO_sm", [128, 64], F32)
    tmp3 = P.sbuf("MO_tmp3", [128, 4, 32], F32)
    imp = P.sbuf("MO_imp", [128, 32], F32)
    score = P.sbuf("MO_score", [128, 32], F32)
    top8 = P.sbuf("MO_top8", [128, 8], F32)
    negm = P.sbuf("MO_negm", [128, 32], F32)
    negT = P.sbuf("MO_negT", [32, 128], BF16)
    scr = [P.sbuf(f"MO_s{i}", [128, T], F32) for i in range(3)]

    w_in = io['w_in']
    O.dma('sp', bias_c[:], io['bias_c'].t, [io['bias_c']], [bias_c])
    O.dma('sp', bias_s[:], io['bias_s'].t, [io['bias_s']], [bias_s])
    for kv in range(2):
        O.dma('poolq', w1[kv][:], io['w1'].t[kv].rearrange("(l d) e -> d l e", d=128), [io['w1']], [w1[kv]])
        O.dma('poolq', w2[kv][:], io['w2'].t[kv], [io['w2']], [w2[kv]])
        O.dma('sp', posT[:, kv, :], io['pos'].t[kv].rearrange("l d -> d l"), [io['pos']], [posT], allow_slow_non_contiguous=True)
    O.copy('dve', posTb[:], posT[:], [posT], [posTb])
    O.memset('pool', vsa[:, :, 128:129], 1.0, [vsa])
    O.memset('pool', vwa[:, :, 128:129], 1.0, [vwa])
    O.memset('pool', vaug[:], 0.0, [vaug])
    O.memset('pool', PTc[:], 0.0, [PTc])
    compute_uT(P, O, C, io['h_full'], io['g0'], uT, scr, st)
    for kv in range(2):
        b = bkr.get()
        for l in range(32):
            O.mm(b[:, 0:1], w1[kv][:, l, :], posTb[:, kv, l:l + 1], l == 0, l == 31, [w1[kv], posTb], [b])
        O.copy('act', cvec[:, kv:kv + 1], b[:, 0:1], [b], [cvec])
    wsm = ring.load(w_in, 2560, 24)

    def ev_g(q, b):
        O.act(gates[:, 4 * q:4 * q + 4, :], b[:].rearrange("p (j c) -> p j c", j=4)[:, :, 0:24], AF.Sigmoid, [b], [gates])
    proj_tm(O, bk, uT, wsm, 24, ev_g)

    oT_out = io['oT_out']
    for gl in range(2):
        for h in range(4):
            ws = ring.load(w_in, gl * 512 + h * 128)
            proj_fm(O, bk, uT, ws, lambda g, b, h=h: O.act(qT[:, h, g * 512:(g + 1) * 512], b[:], AF.Copy, [b], [qT.units[h]],
                                                          scale=float(128 ** -0.5)))
        for dst, col in ((kcT, 1024), (vcT, 1280), (ksT, 1536), (kwT, 2048)):
            ws = ring.load(w_in, col + gl * 128)
            proj_fm(O, bk, uT, ws, lambda g, b, dst=dst: O.copy('act', dst[:, g * 512:(g + 1) * 512], b[:], [b], [dst]))
        for dst, col in ((vsa, 1792), (vwa, 2304)):
            ws = ring.load(w_in, col + gl * 128)
            proj_tm(O, bk, uT, ws, 128, lambda q, b, dst=dst: O.copy('act', dst[:, 4 * q:4 * q + 4, 0:128],
                                                                     b[:].rearrange("p (j c) -> p j c", j=4), [b], [dst]))
        for kv, src in ((0, kcT), (1, vcT)):
            b = bkr.get()
            sv = src[:].rearrange("p (n s) -> p n s", s=16)
            for l in range(32):
                rhs = sv[:, (l // 16):(l // 16) + 127, l % 16]
                O.mm(b[:, 0:127], w1[kv][:, l, :], rhs, l == 0, l == 31, [w1[kv], src], [b])
            O.act(h1[kv][:, 0:127], b[:, 0:127], AF.Silu, [b, cvec], [h1[kv]], bias=cvec[:, kv:kv + 1])
        b = bkr.get()
        O.mm(b[:, 0:127], w2[0][:], h1[0][:, 0:127], True, True, [w2[0], h1[0]], [b])
        O.copy('act', kcmpT[:, 0:127], b[:, 0:127], [b], [kcmpT])
        b = bkr.get()
        O.mm(b[0:127, 0:128], h1[1][:, 0:127], w2[1][:], True, True, [w2[1], h1[1]], [b])
        O.copy('act', vaug[0:127, 0:128], b[0:127, 0:128], [b], [vaug])
        O.dma('sp', vaug[0:127, 128:161], cst['ovl'].t, [cst['ovl']], [vaug])

        for tb in range(NT):
            tsl = slice(tb * 128, (tb + 1) * 128)
            nn = min(8 * tb + 7, 127)
            s0 = 129 - 8 * tb
            pS = bkr.get()
            for h in range(4):
                O.mm(pS[0:nn, h * 128:(h + 1) * 128], kcmpT[:, 0:nn], qT[:, h, tsl], True, False, [kcmpT, qT.units[h]], [pS])
                O.mm(pS[0:nn, h * 128:(h + 1) * 128], cst['zwide'][0:8, s0:s0 + nn], cst['cmask'][0:8, :], False, True,
                     [cst['zwide'], cst['cmask']], [pS])
            for h in range(4):
                O.act(PTc[0:nn, h, :], pS[0:nn, h * 128:(h + 1) * 128], AF.Exp, [pS, bias_c], [PTc],
                      bias=bias_c[0:nn, gl * 4 + h, tb:tb + 1])
            pR = [bkr.get(), bkr.get()]
            for h in range(4):
                O.mm(pR[h // 2][:, (h % 2) * 161:(h % 2) * 161 + 161], PTc[0:nn, h, :], vaug[0:nn, :], True, True,
                     [PTc, vaug], [pR[h // 2]])
            rv = [pR[i][:, 0:322].rearrange("p (h c) -> p h c", h=2) for i in range(2)]
            for i in range(2):
                O.copy('dve', sm[:, 2 * i:2 * i + 2], rv[i][:, :, 128], [pR[i]], [sm])
            O.ts('dve', sm[:, 0:4], sm[:, 0:4], 1e-30, None, ALU.max, None, [sm], [sm])
            O.P.op('dve', lambda e: e.reciprocal(out=sm[:, 4:8], in_=sm[:, 0:4]), reads=[sm.u], writes=[sm.u])
            gv = gates[:, tb, gl * 12:gl * 12 + 12].rearrange("p (h j) -> p h j", j=3)
            O.tt('dve', sm[:, 8:12], sm[:, 4:8], gv[:, :, 0], ALU.mult, [sm, gates], [sm])
            for h in range(4):
                O.ts('dve', oacc[:, h, :], rv[h // 2][:, h % 2, 0:128], sm[:, 8 + h:9 + h], None, ALU.mult, None,
                     [pR[h // 2], sm], [oacc])
            for i in range(2):
                O.tt('dve', tmp3[:, 2 * i:2 * i + 2, :], rv[i][:, :, 129:161],
                     sm[:, 4 + 2 * i:6 + 2 * i].unsqueeze(2).to_broadcast([128, 2, 32]), ALU.mult, [pR[i], sm], [tmp3])
            O.P.op('dve', lambda e: e.tensor_reduce(out=imp[:], in_=tmp3[:].rearrange("p h i -> p i h"), axis=AX.X, op=ALU.add),
                   reads=[tmp3.u], writes=[imp.u])
            O.tt('dve', score[:], imp[:], cst['keepw'][:, 31 - 2 * tb:63 - 2 * tb], ALU.mult, [imp, cst['keepw']], [score])
            O.tt('dve', score[:], score[:], cst['addw'][:, 31 - 2 * tb:63 - 2 * tb], ALU.add, [score, cst['addw']], [score])
            O.memset('dve', score[:, 0:1], 1.0e4, [score])
            O.P.op('dve', lambda e: e.max(out=top8[:], in_=score[:]), reads=[score.u], writes=[top8.u])
            O.ts('dve', negm[:], score[:], top8[:, 7:8], None, ALU.is_ge, None, [score, top8], [negm])
            O.ts('dve', negm[:], negm[:], -1.0, 30000.0, ALU.add, ALU.mult, [negm], [negm])
            pT = bkr.get()
            O.tr(pT[0:32, 0:128], negm[:], identf[:], [negm, identf], [pT])
            O.copy('act', negT[:], pT[0:32, 0:128], [pT], [negT])
            accS = [pb[0], pb[1], pb[2], pb[3]]
            accW = accS
            ipt = 0
            for br, kT_, va, acc, kbs in ((0, ksT, vsa, accS, list(range(0, tb + 1))),
                                          (1, kwT, vwa, accW, list(range(max(0, tb - 4), tb + 1)))):
                for kb in kbs:
                    ksl = slice(kb * 128, (kb + 1) * 128)
                    delta = tb - kb
                    pq = bkr.get()
                    extra = []
                    if br == 0:
                        extra.append((cst['wsel'][0:32, ksl], negT[:], [cst['wsel'], negT]))
                    if delta == 0:
                        extra.append((ident[:], cst['causal'][:], [ident, cst['causal']]))
                    if br == 1 and delta == 4:
                        extra.append((ident[:], cst['anti'][:], [ident, cst['anti']]))
                    O.mm(pq[:].rearrange("p (h t) -> p h t", h=4), kT_[:, ksl], qT[:, :, tsl], True, len(extra) == 0, [kT_, qT], [pq])
                    for ei, (l_, r_, rd) in enumerate(extra):
                        kk = r_.shape[0]
                        O.mm(pq[:].rearrange("p (h t) -> p h t", h=4), l_, r_.unsqueeze(1).to_broadcast([kk, 4, 128]),
                             False, ei == len(extra) - 1, rd, [pq])
                    PT = PTs[ipt % 6]
                    ipt += 1
                    for h in range(4):
                        O.act(PT[:, h, :], pq[:, h * 128:(h + 1) * 128], AF.Exp, [pq, bias_s], [PT],
                              bias=bias_s[:, gl * 4 + h, delta:delta + 1])
                    for h in range(4):
                        O.mm(acc[h][:, 0:129], PT[:, h, :], va[:, kb, :],
                             kb == kbs[0], kb == kbs[-1], [PT, va], [acc[h]])
                for h in range(4):
                    O.copy('dve', sm[:, 16 + h:17 + h], acc[h][:, 128:129], [acc[h]], [sm])
                O.P.op('dve', lambda e: e.reciprocal(out=sm[:, 20:24], in_=sm[:, 16:20]), reads=[sm.u], writes=[sm.u])
                O.tt('dve', sm[:, 24:28], sm[:, 20:24], gv[:, :, br + 1], ALU.mult, [sm, gates], [sm])
                for h in range(4):
                    O.stt('dve', oacc[:, h, :], acc[h][:, 0:128], sm[:, 24 + h:25 + h], oacc[:, h, :],
                          ALU.mult, ALU.add, [acc[h], sm, oacc], [oacc])
            O.copy('act', oab[:], oacc[:], [oacc], [oab])
            pO = bkr.get()
            pOb = pO.t.ap().bitcast(BF16)
            for h in range(4):
                O.tr(pOb[:, h * 128:(h + 1) * 128], oab[:, h, :], ident[:], [oab, ident], [pO])
            for h in range(4):
                O.copy('act', oTs[h][:, tsl], pOb[:, h * 128:(h + 1) * 128], [pO], [oTs[h]])
        for h in range(4):
            r0 = (gl * 4 + h) * 128
            O.dma('sp', oT_out.t[r0:r0 + 128, :], oTs[h][:], [oTs[h]], [oT_out], is_output=io.get('final', False))
    P.barrier()
    P.sbuf_reset(mark)


BF = ml_dtypes.bfloat16
NEG = -1.0e5

def me_const_arrays():
    idx = np.arange(128)
    same = idx[:, None] // 64 == idx[None, :] // 64
    d = {}
    d['maskT'] = (same & (idx[:, None] <= idx[None, :])).astype(BF)
    d['mstrict'] = np.where(same & (idx[None, :] < idx[:, None]), 0.0, NEG).astype(np.float32)
    d['mincl'] = np.where(same & (idx[:, None] <= idx[None, :]), 0.0, NEG).astype(np.float32)
    d['tri'] = (same & (idx[:, None] <= idx[None, :])).astype(np.float32)
    d['blk'] = same.astype(np.float32)
    d['ch0'] = np.repeat((idx < 64)[:, None], 128, 1).astype(np.float32)
    d['ch1'] = np.repeat((idx >= 64)[:, None], 128, 1).astype(np.float32)
    d['onesf'] = np.ones((128, 128), np.float32)
    d['onesb'] = np.ones((128, 128), BF)
    rst = np.ones((128, 2048), np.float32); rst[:, ::64] = 0
    d['rst'] = rst.astype(BF)
    d['m01'] = np.stack([(idx < 64), (idx >= 64)], 1).astype(np.float32)
    return d

ME_DT = {'maskT': BF16, 'onesb': BF16, 'rst': BF16}

def common_arrays():
    return {'ident_bf': np.eye(128).astype(BF), 'ident_f': np.eye(128, dtype=np.float32)}

def declare_consts(nc, arrays, dts, prefix):
    out = {}
    for k, v in arrays.items():
        dt = dts.get(k, F32)
        out[k] = Buf(nc.dram_tensor(prefix + k, list(v.shape), dt, kind="ExternalInput").ap(), prefix + k)
    return out

def load_consts(P, O, cd, prefix):
    out = {}
    for k, b in cd.items():
        sb = P.sbuf(prefix + k + "_sb", list(b.t.shape), b.t.dtype)
        O.dma('sp', sb[:], b.t, [b], [sb])
        out[k] = sb
    return out


def mo_const_arrays():
    NEGB = -30000.0
    c = {}
    n = np.arange(127); i = np.arange(32)
    ovl = ((16 * n[:, None] < 64 * i[None, :] + 64) & (16 * n[:, None] + 32 > 64 * i[None, :])).astype(np.float32)
    c['ovl'] = np.concatenate([np.ones((127, 1), np.float32), ovl], 1).astype(BF)
    tt = np.arange(128); k = np.arange(8)
    c['cmask'] = np.where(16 * (k[:, None] - 1) + 31 <= tt[None, :], 0.0, NEGB).astype(BF)
    zw = np.zeros((8, 256), np.float32); zw[k, k + 128] = 1.0
    c['zwide'] = zw.astype(BF)
    jj = np.arange(64); ip = jj - 31
    half = (tt >= 64).astype(int)
    forced = (ip[None, :] == half[:, None]) | (ip[None, :] == half[:, None] - 1)
    future = ip[None, :] > half[:, None]
    c['keepw'] = (~(forced | future)).astype(np.float32)
    c['addw'] = np.where(forced, 1e4, np.where(future, -1e4, 0.0)).astype(np.float32)
    ss = np.arange(128)
    c['causal'] = np.where(ss[:, None] <= tt[None, :], 0.0, NEGB).astype(BF)
    c['anti'] = np.where(ss[:, None] > tt[None, :], 0.0, NEGB).astype(BF)
    wsel = np.zeros((32, 2048), np.float32); wsel[np.arange(2048) // 64, np.arange(2048)] = 1.0
    c['wsel'] = wsel.astype(BF)
    return c

MO_DT = {'ovl': BF16, 'cmask': BF16, 'zwide': BF16, 'causal': BF16, 'anti': BF16, 'wsel': BF16}

def mo_bias_arrays(hh):
    slopes = (2.0 ** (-8.0 * np.arange(1, 17) / 16))[8 * hh:8 * hh + 8]
    p = np.arange(128)
    tb = np.arange(16)
    bc = slopes[None, :, None] * (16 * p[:, None, None] + 31 - 128 * tb[None, None, :] - 64)
    bs = slopes[None, :, None] * (p[:, None, None] - 64 - 128 * tb[None, None, :])
    return {'bias_c': bc.astype(np.float32), 'bias_s': bs.astype(np.float32)}


from concourse.bass_utils import run_bass_kernel_spmd


def _dram(nc, name, shape, dt, kind):
    return Buf(nc.dram_tensor(name, list(shape), dt, kind=kind).ap(), name)


def _me_inputs_for_core(inp, j, hh):
    w = inp['ab_w_in'][j]
    s = lambda base: w[:, base + 512 * hh: base + 512 * hh + 512]
    w_core = np.concatenate([s(0), s(1024), s(2048), s(3072), s(4096), s(5120), s(6144), s(7168),
                             w[:, 8192 + 4 * hh:8192 + 4 * hh + 4], w[:, 8200 + 4 * hh:8200 + 4 * hh + 4]], 1)
    lbl = inp['hgrn_lb_logits'].reshape(2, 8, 128)[:, 4 * hh:4 * hh + 4, :].transpose(2, 0, 1)
    cw = inp['gdn_conv_w'][j].reshape(4, 3, 8, 128)[:, :, 4 * hh:4 * hh + 4, :].transpose(3, 1, 2, 0).reshape(128, 12, 4)
    return {'w_in': np.ascontiguousarray(w_core), 'lbl': np.ascontiguousarray(lbl), 'conv_w': np.ascontiguousarray(cw),
            'a_log': np.ascontiguousarray(inp['gdn_a_log'][j][4 * hh:4 * hh + 4]),
            'dt_bias': np.ascontiguousarray(inp['gdn_dt_bias'][j][4 * hh:4 * hh + 4]),
            'hg': np.ascontiguousarray(inp['hgrn_norm_g'][j]), 'gg': np.ascontiguousarray(inp['gdn_norm_g'][j])}


def _mo_inputs_for_core(inp, j, hh):
    w = inp['nsa_w_in'][j]
    parts = [w[:, 1024 * hh:1024 * hh + 1024]]
    for b in range(6):
        base = 2048 + 512 * b
        parts.append(w[:, base + 256 * hh: base + 256 * hh + 256])
    parts.append(w[:, 5120 + 24 * hh:5120 + 24 * hh + 24])
    d = {'w_in': np.ascontiguousarray(np.concatenate(parts, 1)), 'pos': np.ascontiguousarray(inp['nsa_cmp_pos'][j]),
         'w1': np.ascontiguousarray(inp['nsa_cmp_w1'][j]), 'w2': np.ascontiguousarray(inp['nsa_cmp_w2'][j])}
    d.update(mo_bias_arrays(hh))
    return d


def _build_me_prog(layer):
    nc = bass.Bass("TRN2", target_bir_lowering=False)
    P = Prog(nc); O = Ops(P)
    cd = declare_consts(nc, common_arrays(), {'ident_bf': BF16}, "")
    mcd = declare_consts(nc, me_const_arrays(), ME_DT, "mec_")
    g0 = _dram(nc, 'g0', [2048], F32, "ExternalInput")
    io = {'h_full': _dram(nc, 'h_full', [2048, 2048], F32, "ExternalInput"), 'g0': g0.t,
          'w_in': _dram(nc, 'w_in', [2048, 4104], F32, "ExternalInput"),
          'lbl': _dram(nc, 'lbl', [128, 2, 4], F32, "ExternalInput"),
          'conv_w': _dram(nc, 'conv_w', [128, 12, 4], F32, "ExternalInput"),
          'a_log': _dram(nc, 'a_log', [4], F32, "ExternalInput"), 'dt_bias': _dram(nc, 'dt_bias', [4], F32, "ExternalInput"),
          'hg': _dram(nc, 'hg', [128], F32, "ExternalInput"), 'gg': _dram(nc, 'gg', [128], F32, "ExternalInput"),
          'oT_out': _dram(nc, 'oT_out', [1024, 2048], BF16, "ExternalOutput"), 'final': True}
    C = alloc_common(P)
    load_common(P, O, C, cd)
    C['me_const'] = load_consts(P, O, mcd, "mec_")
    build_ME(P, O, C, io, layer)
    P.emit()
    return nc


def _build_mo_prog():
    nc = bass.Bass("TRN2", target_bir_lowering=False)
    P = Prog(nc); O = Ops(P)
    cd = declare_consts(nc, common_arrays(), {'ident_bf': BF16}, "")
    mcd = declare_consts(nc, mo_const_arrays(), MO_DT, "moc_")
    g0 = _dram(nc, 'g0', [2048], F32, "ExternalInput")
    io = {'h_full': _dram(nc, 'h_full', [2048, 2048], F32, "ExternalInput"), 'g0': g0.t,
          'w_in': _dram(nc, 'w_in', [2048, 2584], F32, "ExternalInput"),
          'pos': _dram(nc, 'pos', [2, 32, 128], F32, "ExternalInput"),
          'w1': _dram(nc, 'w1', [2, 4096, 128], F32, "ExternalInput"),
          'w2': _dram(nc, 'w2', [2, 128, 128], F32, "ExternalInput"),
          'bias_c': _dram(nc, 'bias_c', [128, 8, 16], F32, "ExternalInput"),
          'bias_s': _dram(nc, 'bias_s', [128, 8, 16], F32, "ExternalInput"),
          'oT_out': _dram(nc, 'oT_out', [1024, 2048], BF16, "ExternalOutput"), 'final': True}
    C = alloc_common(P)
    load_common(P, O, C, cd)
    ovl = mcd.pop('ovl')
    C['mo_const'] = load_consts(P, O, mcd, "moc_")
    C['mo_const']['ovl'] = ovl
    build_MO(P, O, C, io)
    P.emit()
    return nc


def _build_f_prog():
    nc = bass.Bass("TRN2", target_bir_lowering=False)
    P = Prog(nc); O = Ops(P)
    cd = declare_consts(nc, common_arrays(), {'ident_bf': BF16}, "")
    io = {'h_in': _dram(nc, 'h_in', [1024, D], F32, "ExternalInput"),
          'oT': _dram(nc, 'oT', [D, 1024], BF16, "ExternalInput"),
          'w_out': _dram(nc, 'w_out', [D, D], F32, "ExternalInput"),
          'g': _dram(nc, 'g', [3, D], F32, "ExternalInput"),
          'w_gu': _dram(nc, 'w_gu', [D, 2 * DFF], F32, "ExternalInput"),
          'w_down': _dram(nc, 'w_down', [DFF, D], F32, "ExternalInput"),
          'h_out': _dram(nc, 'h_out', [1024, D], F32, "ExternalOutput"), 'final': True}
    C = alloc_common(P)
    load_common(P, O, C, cd)
    build_F(P, O, C, io)
    P.emit()
    return nc


def kernel(**inputs):
    inp = {k: np.ascontiguousarray(np.asarray(v)) for k, v in inputs.items()}
    h = inp['x'].astype(np.float32).copy()
    cores = list(range(8))
    com = common_arrays()
    mec = {"mec_" + k: v for k, v in me_const_arrays().items()}
    moc = {"moc_" + k: v for k, v in mo_const_arrays().items()}
    for layer in range(4):
        j = layer // 2
        g0 = np.ascontiguousarray(inp['norm_g'][layer, 0])
        maps = []
        for c in cores:
            b, hh = c // 2, c % 2
            if layer % 2 == 0:
                m = _me_inputs_for_core(inp, j, hh); m.update(mec)
            else:
                m = _mo_inputs_for_core(inp, j, hh); m.update(moc)
            m.update(com)
            m['h_full'] = np.ascontiguousarray(h[b]); m['g0'] = g0
            maps.append(m)
        nc = _build_me_prog(layer) if layer % 2 == 0 else _build_mo_prog()
        res = run_bass_kernel_spmd(nc, maps, core_ids=cores).results
        oT_full = np.zeros((4, 2048, 2048), dtype=res[0]['oT_out'].dtype)
        for c in cores:
            b, hh = c // 2, c % 2
            o = res[c]['oT_out']
            if layer % 2 == 0:
                oT_full[b, 512 * hh:512 * hh + 512] = o[0:512]
                oT_full[b, 1024 + 512 * hh:1024 + 512 * hh + 512] = o[512:1024]
            else:
                oT_full[b, 1024 * hh:1024 * hh + 1024] = o
        w_out = inp['ab_w_out'][j] if layer % 2 == 0 else inp['nsa_w_out'][j]
        maps = []
        for c in cores:
            b, th = c // 2, c % 2
            m = {'h_in': np.ascontiguousarray(h[b, 1024 * th:1024 * th + 1024]),
                 'oT': np.ascontiguousarray(oT_full[b][:, 1024 * th:1024 * th + 1024]),
                 'w_out': w_out, 'g': np.ascontiguousarray(inp['norm_g'][layer, 1:4]),
                 'w_gu': inp['ffn_w_gu'][layer], 'w_down': inp['ffn_w_down'][layer]}
            m.update(com)
            maps.append(m)
        nc = _build_f_prog()
        res = run_bass_kernel_spmd(nc, maps, core_ids=cores).results
        for c in cores:
            b, th = c // 2, c % 2
            h[b, 1024 * th:1024 * th + 1024] = res[c]['h_out']
    return h
```
